# Optimizing a Trainium2 kernel written in Bass

```python
import math
import jax, jax.numpy as jnp
from jax import lax
import numpy as np

D_MODEL = 1024
BATCH = 16
SEQ = 4096
DEPTH = 2

CHUNK = 64
HEAD_DIM = 64
D_MIX = D_MODEL
N_GH = 4
GROUP_W = N_GH * HEAD_DIM
IDX_HEADS = 8
IDX_DIM = 64
TOPK_MAX = 256
RET_THETA = 10000.0
GLA_DK = HEAD_DIM // 2
GLA_RANK = 16
GLA_TAU = 16.0
DIFF_DK = HEAD_DIM // 2
ROPE_THETA = 500000.0
ROPE_FRAC = 4
D_FF = 4 * D_MODEL
Q_BLOCK = 128
EPS = 1e-6
NEG_INF = -1e30

IN_SPLITS = (
    GROUP_W, HEAD_DIM, HEAD_DIM, IDX_HEADS * IDX_DIM, IDX_DIM, IDX_HEADS,
    GROUP_W, GROUP_W, GROUP_W, GROUP_W,
    N_GH * GLA_DK, N_GH * GLA_DK, GROUP_W, GLA_RANK, GROUP_W,
    2 * N_GH * DIFF_DK, 2 * N_GH * DIFF_DK, GROUP_W,
)
IN_COLS = sum(IN_SPLITS)

kernel_name = "chunk_causal_hybrid_head_group_trunk"


def rms_norm(x, g):
    xf = x.astype(jnp.float32)
    y = xf * lax.rsqrt(jnp.mean(xf * xf, axis=-1, keepdims=True) + EPS)
    return (y * g.astype(jnp.float32)).astype(x.dtype)


def head_norm(y, g, center, dtype):
    B, T, H, D = y.shape
    yf = y.astype(jnp.float32)
    if center:
        yf = yf - jnp.mean(yf, axis=-1, keepdims=True)
    yf = yf * lax.rsqrt(jnp.mean(yf * yf, axis=-1, keepdims=True) + EPS)
    yf = yf * g.astype(jnp.float32).reshape(H, D)
    return yf.reshape(B, T, H * D).astype(dtype)


def rope(x, pos, rot_dims, theta):
    half = rot_dims // 2
    inv = theta ** (-jnp.arange(half, dtype=jnp.float32) / half)
    ang = pos.astype(jnp.float32)[..., None] * inv
    cos = jnp.cos(ang)[:, :, None, :].astype(x.dtype)
    sin = jnp.sin(ang)[:, :, None, :].astype(x.dtype)
    x1, x2, xp = x[..., :half], x[..., half:rot_dims], x[..., rot_dims:]
    return jnp.concatenate([x1 * cos - x2 * sin, x1 * sin + x2 * cos, xp], axis=-1)


def split_blocks(a, size):
    B, T = a.shape[:2]
    return jnp.moveaxis(a.reshape((B, T // size, size) + a.shape[2:]), 1, 0)


def merge_blocks(a):
    N, B, S = a.shape[:3]
    return jnp.moveaxis(a, 0, 1).reshape((B, N * S) + a.shape[3:])


def dsa_mixer(q, k, v, qi, ki, wi, pos):
    B, T = q.shape[:2]
    topk = min(TOPK_MAX, T // 4)
    q = rope(q, pos, HEAD_DIM // ROPE_FRAC, ROPE_THETA) * (HEAD_DIM ** -0.5)
    k = rope(k[:, :, None], pos, HEAD_DIM // ROPE_FRAC, ROPE_THETA)[:, :, 0]
    qi = rope(qi, pos, IDX_DIM // ROPE_FRAC, ROPE_THETA)
    ki = rope(ki[:, :, None], pos, IDX_DIM // ROPE_FRAC, ROPE_THETA)[:, :, 0]
    wi = wi * (IDX_HEADS ** -0.5 * IDX_DIM ** -0.5)
    key_chunk = jnp.arange(T) // CHUNK
    gather = jax.vmap(lambda tab, idx: tab[idx])

    def block(args):
        qb, qib, wib, blk = args
        q_chunk = (blk * Q_BLOCK + jnp.arange(Q_BLOCK)) // CHUNK
        admissible = key_chunk[None, :] <= q_chunk[:, None]
        logits = jnp.einsum("bqhd,bsd->bqhs", qib, ki)
        score = jnp.einsum("bqh,bqhs->bqs", wib, jax.nn.relu(logits)).astype(jnp.float32)
        score = jnp.where(admissible[None], score, NEG_INF)
        _, sel = lax.top_k(score, topk)
        valid = (sel // CHUNK) <= q_chunk[None, :, None]
        k_sel = gather(k, sel)
        v_sel = gather(v, sel)
        s = jnp.einsum("bqhd,bqkd->bqhk", qb, k_sel).astype(jnp.float32)
        s = jnp.where(valid[:, :, None, :], s, NEG_INF)
        p = jax.nn.softmax(s, axis=-1).astype(v.dtype)
        return jnp.einsum("bqhk,bqkd->bqhd", p, v_sel)

    out = lax.map(block, (split_blocks(q, Q_BLOCK), split_blocks(qi, Q_BLOCK),
                          split_blocks(wi, Q_BLOCK), jnp.arange(T // Q_BLOCK)))
    return merge_blocks(out)


def retention_mixer(q, k, v, pos):
    B, T, H, D = q.shape
    f32 = jnp.float32
    q = (rope(q, pos, HEAD_DIM, RET_THETA) * (HEAD_DIM ** -0.5)).astype(f32)
    k = rope(k, pos, HEAD_DIM, RET_THETA).astype(f32)
    v = v.astype(f32)
    log_g = jnp.log(1.0 - 2.0 ** (-5.0 - jnp.arange(H, dtype=f32)))
    i = jnp.arange(CHUNK, dtype=f32)
    d_intra = jnp.exp(jnp.abs(i[:, None] - i[None, :])[None] * log_g[:, None, None])
    xi = jnp.exp((i + 1.0)[:, None] * log_g[None, :])[None, :, :, None]
    zeta = jnp.exp((CHUNK - 1.0 - i)[:, None] * log_g[None, :])[None, :, :, None]
    g_chunk = jnp.exp(CHUNK * log_g)[None, :, None, None]

    def step(R, xs):
        qc, kc, vc = xs
        s = jnp.einsum("bihd,bjhd->bhij", qc, kc) * d_intra[None]
        intra = jnp.einsum("bhij,bjhd->bihd", s, vc)
        inter = jnp.einsum("bihd,bhde->bihe", qc, R) * xi
        R = g_chunk * R + jnp.einsum("bjhd,bjhe->bhde", kc * zeta, vc)
        return R, intra + inter

    R0 = jnp.zeros((B, H, D, D), f32)
    _, out = lax.scan(step, R0, (split_blocks(q, CHUNK), split_blocks(k, CHUNK),
                                 split_blocks(v, CHUNK)))
    return merge_blocks(out)


def gla_mixer(q, k, v, a_low, w_a2, b_a):
    B, T, H, DV = v.shape
    f32 = jnp.float32
    log_a = jax.nn.log_sigmoid((jnp.einsum("btr,rk->btk", a_low, w_a2) + b_a).astype(f32)) / GLA_TAU
    log_a = log_a.reshape(B, T, H, GLA_DK)
    q = (q * (GLA_DK ** -0.5)).astype(f32)
    k = k.astype(f32)
    v = v.astype(f32)

    def step(S, xs):
        qc, kc, vc, lac = xs
        b = jnp.cumsum(lac, axis=1)
        decay = jnp.exp(-jnp.abs(b[:, :, None] - b[:, None, :]))
        A = jnp.einsum("bthk,btshk,bshk->bhts", qc, decay, kc)
        intra = jnp.einsum("bhts,bshd->bthd", A, vc)
        inter = jnp.einsum("bthk,bhkd->bthd", qc * jnp.exp(b), S)
        b_last = b[:, -1]
        S = jnp.exp(b_last)[..., None] * S + jnp.einsum(
            "bshk,bshd->bhkd", kc * jnp.exp(b_last[:, None] - b), vc)
        return S, intra + inter

    S0 = jnp.zeros((B, H, GLA_DK, DV), f32)
    _, out = lax.scan(step, S0, (split_blocks(q, CHUNK), split_blocks(k, CHUNK),
                                 split_blocks(v, CHUNK), split_blocks(log_a, CHUNK)))
    return merge_blocks(out)


def diff_mixer(q, k, v, pos, lam_q1, lam_k1, lam_q2, lam_k2, lam_init):
    B, T = q.shape[:2]
    f32 = jnp.float32
    q = rope(q, pos, DIFF_DK // ROPE_FRAC, ROPE_THETA) * (DIFF_DK ** -0.5)
    k = rope(k, pos, DIFF_DK // ROPE_FRAC, ROPE_THETA)
    q = q.reshape(B, T, N_GH, 2, DIFF_DK)
    k = k.reshape(B, T, N_GH, 2, DIFF_DK)
    lam = (jnp.exp(jnp.sum(lam_q1.astype(f32) * lam_k1.astype(f32)))
           - jnp.exp(jnp.sum(lam_q2.astype(f32) * lam_k2.astype(f32))) + lam_init)
    key_chunk = jnp.arange(T) // CHUNK

    def block(args):
        qb, blk = args
        q_chunk = (blk * Q_BLOCK + jnp.arange(Q_BLOCK)) // CHUNK
        mask = key_chunk[None, :] <= q_chunk[:, None]
        s = jnp.einsum("bqhcd,bshcd->bhcqs", qb, k).astype(f32)
        s = jnp.where(mask[None, None, None], s, NEG_INF)
        p = jax.nn.softmax(s, axis=-1)
        w = p[:, :, 0] - lam * p[:, :, 1]
        return jnp.einsum("bhqs,bshd->bqhd", w.astype(v.dtype), v)

    out = lax.map(block, (split_blocks(q, Q_BLOCK), jnp.arange(T // Q_BLOCK)))
    return merge_blocks(out)


def hybrid_layer(x, cond, pos, layer_idx, mod_w, mod_b, attn_pre_g, attn_post_g,
                 mlp_pre_g, mlp_post_g, w_in, gla_wa2, gla_ba, lam_q1, lam_k1,
                 lam_q2, lam_k2, head_norm_g, w_out, mlp_w1, mlp_w2):
    B, T, _ = x.shape
    dt = x.dtype
    mod = (cond @ mod_w + mod_b)[:, None, :]
    shift1, scale1, gate1, shift2, scale2, gate2 = jnp.split(mod, 6, axis=-1)

    h = rms_norm(x, attn_pre_g) * (1.0 + scale1) + shift1
    proj = h @ w_in
    (a_q, a_k, a_v, a_qi, a_ki, a_wi, b_q, b_k, b_v, b_g,
     c_q, c_k, c_v, c_a, c_g, d_q, d_k, d_v) = jnp.split(
        proj, np.cumsum(IN_SPLITS)[:-1].tolist(), axis=-1)
    heads = lambda a, d: a.reshape(B, T, -1, d)
    g_a, g_b, g_c, g_d = jnp.split(head_norm_g, 4)
    lam_init = 0.8 - 0.6 * math.exp(-0.3 * layer_idx)

    y_a = head_norm(dsa_mixer(heads(a_q, HEAD_DIM), a_k, a_v, heads(a_qi, IDX_DIM),
                              a_ki, a_wi, pos), g_a, False, dt)
    y_b = jax.nn.silu(b_g) * head_norm(retention_mixer(
        heads(b_q, HEAD_DIM), heads(b_k, HEAD_DIM), heads(b_v, HEAD_DIM), pos), g_b, True, dt)
    y_c = jax.nn.silu(c_g) * head_norm(gla_mixer(
        heads(c_q, GLA_DK), heads(c_k, GLA_DK), heads(c_v, HEAD_DIM), c_a, gla_wa2, gla_ba),
        g_c, True, dt)
    y_d = head_norm(diff_mixer(heads(d_q, DIFF_DK), heads(d_k, DIFF_DK), heads(d_v, HEAD_DIM),
                               pos, lam_q1, lam_k1, lam_q2, lam_k2, lam_init),
                    g_d, False, dt) * (1.0 - lam_init)
    y = jnp.concatenate([y_a, y_b, y_c, y_d], axis=-1) @ w_out
    x = x + gate1 * rms_norm(y, attn_post_g)

    h = rms_norm(x, mlp_pre_g) * (1.0 + scale2) + shift2
    y = jnp.square(jax.nn.relu(h @ mlp_w1)) @ mlp_w2
    return x + gate2 * rms_norm(y, mlp_post_g)


def setup_inputs(seed: int = 0) -> dict:
    key = jax.random.key(seed)
    ks = jax.random.split(key, 20)
    nrm = lambda k, shape, s: jax.random.normal(k, shape, jnp.float32) * s
    gain = lambda k, shape: 1.0 + 0.02 * jax.random.normal(k, shape, jnp.float32)
    x = nrm(ks[0], (BATCH, SEQ, D_MODEL), 1.0)
    c = nrm(ks[1], (BATCH, D_MODEL), 1.0)
    offset = jax.random.randint(ks[2], (BATCH, 1), 0, 64, dtype=jnp.int32) * CHUNK
    positions = offset + jnp.arange(SEQ, dtype=jnp.int32)[None, :]
    return {
        "x": x,
        "c": c,
        "positions": positions,
        "mod_w": nrm(ks[3], (DEPTH, D_MODEL, 6 * D_MODEL), 0.5 * D_MODEL ** -0.5),
        "mod_b": nrm(ks[4], (DEPTH, 6 * D_MODEL), 0.01),
        "attn_pre_g": gain(ks[5], (DEPTH, D_MODEL)),
        "attn_post_g": gain(ks[6], (DEPTH, D_MODEL)),
        "mlp_pre_g": gain(ks[7], (DEPTH, D_MODEL)),
        "mlp_post_g": gain(ks[8], (DEPTH, D_MODEL)),
        "w_in": nrm(ks[9], (DEPTH, D_MODEL, IN_COLS), D_MODEL ** -0.5),
        "gla_wa2": nrm(ks[10], (DEPTH, GLA_RANK, N_GH * GLA_DK), GLA_RANK ** -0.5),
        "gla_ba": nrm(ks[11], (DEPTH, N_GH * GLA_DK), 0.1),
        "lam_q1": nrm(ks[12], (DEPTH, DIFF_DK), 0.1),
        "lam_k1": nrm(ks[13], (DEPTH, DIFF_DK), 0.1),
        "lam_q2": nrm(ks[14], (DEPTH, DIFF_DK), 0.1),
        "lam_k2": nrm(ks[15], (DEPTH, DIFF_DK), 0.1),
        "head_norm_g": gain(ks[16], (DEPTH, D_MIX)),
        "w_out": nrm(ks[17], (DEPTH, D_MIX, D_MODEL), D_MIX ** -0.5),
        "mlp_w1": nrm(ks[18], (DEPTH, D_MODEL, D_FF), D_MODEL ** -0.5),
        "mlp_w2": nrm(ks[19], (DEPTH, D_FF, D_MODEL), D_FF ** -0.5),
    }


def reference(x, c, positions, mod_w, mod_b, attn_pre_g, attn_post_g, mlp_pre_g,
              mlp_post_g, w_in, gla_wa2, gla_ba, lam_q1, lam_k1, lam_q2, lam_k2,
              head_norm_g, w_out, mlp_w1, mlp_w2):
    cond = jax.nn.silu(c)
    for l in range(DEPTH):
        x = hybrid_layer(x, cond, positions, l, mod_w[l], mod_b[l], attn_pre_g[l],
                         attn_post_g[l], mlp_pre_g[l], mlp_post_g[l], w_in[l],
                         gla_wa2[l], gla_ba[l], lam_q1[l], lam_k1[l], lam_q2[l],
                         lam_k2[l], head_norm_g[l], w_out[l], mlp_w1[l], mlp_w2[l])
    return x
```

```python
import math
from contextlib import ExitStack

import numpy as np
import concourse.bass as bass
import concourse.mybir as mybir
from concourse.bass_utils import run_bass_kernel_spmd

F32 = mybir.dt.float32
BF16 = mybir.dt.bfloat16
I32 = mybir.dt.int32
AF = mybir.ActivationFunctionType
ALU = mybir.AluOpType
AX = mybir.AxisListType

D = 1024
DFF = 4096
DEPTH = 2
NCORES = 8
EPS = 1e-6
IN_COLS = 3544
NEG = -30000.0


class Buf:
    __slots__ = ("name", "w", "r")

    def __init__(self, name):
        self.name = name
        self.w = None
        self.r = []


class Sched:
    def __init__(self, nc, es):
        self.nc = nc
        self.engs = {"pe": nc.tensor, "act": nc.scalar, "dve": nc.vector,
                     "pool": nc.gpsimd, "sp": nc.sync}
        self.esem = {e: es.enter_context(nc.semaphore("e_" + e)) for e in self.engs}
        self.ecnt = {e: 0 for e in self.engs}
        self.seen = {e: {} for e in self.engs}
        self.dsem = {}
        self.es = es
        self.nins = 0

    def dma_sem(self, name):
        if name not in self.dsem:
            self.dsem[name] = [self.es.enter_context(self.nc.semaphore("d_" + name)), 0]
        return name

    def _wait(self, eng, key, sem, val):
        if val <= 0:
            return
        if self.seen[eng].get(key, 0) >= val:
            return
        self.seen[eng][key] = val
        self.engs[eng].wait_ge(sem, val)

    def _wait_ev(self, eng, ev):
        if ev[0] == "e":
            if ev[1] == eng and eng == "pe":
                return
            self._wait(eng, "e_" + ev[1], self.esem[ev[1]], ev[2])
        else:
            s = self.dsem[ev[1]]
            self._wait(eng, "d_" + ev[1], s[0], s[1])

    def op(self, eng, fn, reads=(), writes=(), dsem=None):
        deps = []
        for b in reads:
            if b.w is not None:
                deps.append(b.w)
        for b in writes:
            if b.w is not None:
                deps.append(b.w)
            deps.extend(b.r)
        for ev in deps:
            self._wait_ev(eng, ev)
        ins = fn(self.engs[eng])
        self.nins += 1
        if dsem is None:
            self.ecnt[eng] += 1
            ev = ("e", eng, self.ecnt[eng])
            ins.then_inc(self.esem[eng], 1)
        else:
            s = self.dsem[dsem]
            s[1] += 16
            ev = ("d", dsem)
            ins.then_inc(s[0], 16)
        for b in writes:
            b.w = ev
            b.r = []
        for b in reads:
            if ev[0] == "e":
                b.r = [r for r in b.r if not (r[0] == "e" and r[1] == ev[1])]
                b.r.append(ev)
            elif ev not in b.r:
                b.r.append(ev)
        return ins

    def barrier(self):
        for eng in self.engs:
            for o in self.engs:
                if o != eng:
                    self._wait(eng, "e_" + o, self.esem[o], self.ecnt[o])
            for name, s in self.dsem.items():
                self._wait(eng, "d_" + name, s[0], s[1])


class T:
    def __init__(self, t, name, nsub=1):
        self.t = t
        self.b = Buf(name)
        self.sub = [Buf(name + "_%d" % i) for i in range(nsub)] if nsub > 1 else None

    def __getitem__(self, idx):
        return self.t[idx]


class Prog:
    def __init__(self, nseq, T_, depth=DEPTH, debug=False):
        self.nseq, self.T, self.depth, self.debug = nseq, T_, depth, debug
        self.NT = T_ // 128
        self.nc = bass.Bass("TRN2", target_bir_lowering=False)
        self.uid = 0

    def cut(self, k):
        import os
        return int(os.environ.get("STOP", "99")) <= k

    def cut2(self, k):
        import os
        return int(os.environ.get("STOP2", "99")) <= k

    def sb(self, es, shape, dt, name, nsub=1):
        self.uid += 1
        return T(es.enter_context(self.nc.sbuf_tensor("%s_%d" % (name, self.uid), list(shape), dt)), name, nsub)

    def ps(self, es, shape, dt, name):
        self.uid += 1
        return T(es.enter_context(self.nc.psum_tensor("%s_%d" % (name, self.uid), list(shape), dt)), name)

    def dram(self, name, shape, dt, kind):
        return self.nc.dram_tensor(name, list(shape), dt, kind=kind).ap()

    def mm(self, out_t, out_ap, lhsT, rhs, start, stop, reads):
        self.S.op("pe", lambda e: e.matmul(out_ap, lhsT=lhsT, rhs=rhs, start=start, stop=stop,
                                           skip_group_check=True),
                  reads=[r.b if isinstance(r, T) else r for r in reads], writes=[out_t.b])

    def tr(self, out_t, out_ap, in_ap, ident_ap, reads):
        self.S.op("pe", lambda e: e.transpose(out_ap, in_ap, ident_ap),
                  reads=[r.b if isinstance(r, T) else r for r in reads], writes=[out_t.b])

    def bl(self, lst):
        return [r.b if isinstance(r, T) else r for r in lst]

    def act(self, out_ap, in_ap, func, reads, writes, bias=None, scale=None, accum_out=None):
        kw = {}
        if bias is not None:
            kw["bias"] = bias
        if scale is not None:
            kw["scale"] = scale
        if accum_out is not None:
            kw["accum_out"] = accum_out
        self.S.op("act", lambda e: e.activation(out=out_ap, in_=in_ap, func=func, **kw),
                  reads=self.bl(reads), writes=self.bl(writes))

    def ts(self, eng, out_ap, in0, s1, s2, op0, op1, reads, writes, accum_out=None):
        kw = {}
        if accum_out is not None:
            kw["accum_out"] = accum_out
        if op1 is None:
            self.S.op(eng, lambda e: e.tensor_scalar(out=out_ap, in0=in0, scalar1=s1, scalar2=None, op0=op0, **kw),
                      reads=self.bl(reads), writes=self.bl(writes))
        else:
            self.S.op(eng, lambda e: e.tensor_scalar(out=out_ap, in0=in0, scalar1=s1, scalar2=s2, op0=op0, op1=op1, **kw),
                      reads=self.bl(reads), writes=self.bl(writes))

    def tt(self, eng, out_ap, in0, in1, op, reads, writes):
        self.S.op(eng, lambda e: e.tensor_tensor(out=out_ap, in0=in0, in1=in1, op=op),
                  reads=self.bl(reads), writes=self.bl(writes))

    def stt(self, out_ap, in0, scalar, in1, op0, op1, reads, writes):
        self.S.op("dve", lambda e: e.scalar_tensor_tensor(out=out_ap, in0=in0, scalar=scalar, in1=in1, op0=op0, op1=op1),
                  reads=self.bl(reads), writes=self.bl(writes))

    def cp(self, eng, out_ap, in_ap, reads, writes):
        if eng == "act":
            self.S.op("act", lambda e: e.copy(out=out_ap, in_=in_ap), reads=self.bl(reads), writes=self.bl(writes))
        else:
            self.S.op(eng, lambda e: e.tensor_copy(out=out_ap, in_=in_ap), reads=self.bl(reads), writes=self.bl(writes))

    def dma(self, q, out_ap, in_ap, sem, reads, writes, **kw):
        self.S.dma_sem(sem)
        self.S.op(q, lambda e: e.dma_start(out=out_ap, in_=in_ap, **kw), reads=self.bl(reads),
                  writes=self.bl(writes), dsem=sem)

    def memset(self, eng, ap, val, writes):
        self.S.op(eng, lambda e: e.memset(ap, val), reads=[], writes=self.bl(writes))

    def build(self):
        nc = self.nc
        nseq, T_ = self.nseq, self.T
        dr = self.dram
        self.x = dr("x", [nseq, T_, D], F32, "ExternalInput")
        self.cT = dr("cT", [128, 8 * nseq], F32, "ExternalInput")
        self.posT = dr("posT", [128, nseq * self.NT], I32, "ExternalInput")
        self.mod_w = dr("mod_w", [DEPTH, D, 6 * D], F32, "ExternalInput")
        self.mod_b = dr("mod_b", [DEPTH, 6 * D], F32, "ExternalInput")
        self.g_attn_pre = dr("attn_pre_g", [DEPTH, D], F32, "ExternalInput")
        self.g_attn_post = dr("attn_post_g", [DEPTH, D], F32, "ExternalInput")
        self.g_mlp_pre = dr("mlp_pre_g", [DEPTH, D], F32, "ExternalInput")
        self.g_mlp_post = dr("mlp_post_g", [DEPTH, D], F32, "ExternalInput")
        self.w_in = dr("w_in", [DEPTH, D, IN_COLS], F32, "ExternalInput")
        self.gla_wa2 = dr("gla_wa2", [DEPTH, 16, 128], F32, "ExternalInput")
        self.gla_ba = dr("gla_ba", [DEPTH, 128], F32, "ExternalInput")
        self.lam = dr("lam", [DEPTH, 4, 32], F32, "ExternalInput")
        self.head_g = dr("head_norm_g", [DEPTH, D], F32, "ExternalInput")
        self.w_out = dr("w_out", [DEPTH, D, D], F32, "ExternalInput")
        self.w1 = dr("mlp_w1", [DEPTH, D, DFF], F32, "ExternalInput")
        self.w2 = dr("mlp_w2", [DEPTH, DFF, D], F32, "ExternalInput")
        self.consts = dr("consts", [128, NCONST], F32, "ExternalInput")
        self.out = dr("out", [nseq, T_, D], F32, "ExternalOutput")
        kind = "ExternalOutput" if self.debug else "Internal"
        self.X1 = dr("X1", [nseq, T_, D], F32, "ExternalInput" if self.stages.get("x1_in") else kind)
        self.XR = dr("XR", [nseq, T_, D], F32, kind)
        self.YAD = dr("YAD", [nseq, T_, 512], BF16, kind)
        self.bX1 = [Buf("X1_%d" % s) for s in range(nseq)]
        self.bXR = [Buf("XR_%d" % s) for s in range(nseq)]
        self.bYAD = [Buf("YAD_%d" % s) for s in range(nseq)]
        self.bOUT = Buf("out")
        self.bIN = Buf("in")

        with ExitStack() as es:
            self.S = Sched(nc, es)
            self.setup_globals(es)
            for l in range(self.depth):
                src = self.x if l == 0 else self.XR
                bsrc = [self.bIN] * nseq if l == 0 else self.bXR
                dst = self.out if l == self.depth - 1 else self.XR
                bdst = [self.bOUT] * nseq if l == self.depth - 1 else self.bXR
                if self.stages.get("p1", True):
                    self.phase1(l, src, bsrc)
                if self.stages.get("p2", True):
                    self.phase2(l, src, bsrc)
                if self.stages.get("p3", True):
                    self.phase3(l, dst, bdst)
            self.S.barrier()
        return nc

    stages = {}

    def setup_globals(self, es):
        S = self.S
        self.cst = self.sb(es, [128, 128], F32, "cst")
        self.dma("sp", self.cst[:, :], self.consts[:, 0:128], "g0", [], [self.cst])
        self.identb = self.sb(es, [128, 128], BF16, "identb")
        self.cp("dve", self.identb[:, :], self.cst[:, C_IDENT:C_IDENT + 128], [self.cst], [self.identb])
        self.identf = self.sb(es, [128, 128], F32, "identf")
        self.cp("dve", self.identf[:, :], self.cst[:, C_IDENT:C_IDENT + 128], [self.cst], [self.identf])
        cin = self.sb(es, [128, 8 * self.nseq], F32, "cin")
        self.dma("sp", cin[:, :], self.cT, "g0", [], [cin])
        cond = self.sb(es, [128, 8 * self.nseq], F32, "cond")
        self.act(cond[:, :], cin[:, :], AF.Silu, [cin], [cond])
        ones = self.sb(es, [128, 128], F32, "ones")
        self.memset("dve", ones[:, :], 1.0, [ones])
        self.condrep = []
        for s in range(self.nseq):
            cr = self.sb(es, [128, 8, 128], F32, "condrep%d" % s)
            for kc in range(8):
                i = kc * self.nseq + s
                self.ts("dve", cr[:, kc, :], ones[:, :], cond[:, i:i + 1], None, ALU.mult, None, [ones, cond], [cr])
            self.condrep.append(cr)
        self.ones = ones
        self.oneb = self.sb(es, [128, 1], F32, "oneb")
        self.memset("dve", self.oneb[:, :], 1.0, [self.oneb])
        posi = self.sb(es, [128, self.nseq * self.NT], I32, "posi")
        self.dma("sp", posi[:, :], self.posT, "g0", [], [posi])
        self.posf = self.sb(es, [128, self.nseq * self.NT], F32, "posf")
        self.cp("dve", self.posf[:, :], posi[:, :], [posi], [self.posf])

    def compute_mod(self, es_tmp, l, s, col0, outs, psum_tiles):
        mws = [self.sb(es_tmp, [128, 8, 128], F32, "mw%d" % k) for k in range(2)]
        mb = self.sb(es_tmp, [128, 512], F32, "mb")
        mcnt = 0
        mwv = self.mod_w[l].rearrange("(kc p) n -> p kc n", p=128)
        j = 0
        for oi, dest in enumerate(outs):
            for hf in range(2):
                c0 = col0 + oi * 1024 + hf * 512
                pst = psum_tiles[j % len(psum_tiles)]
                j += 1
                self.dma("sp", mb[:, :], self.mod_b[l, c0:c0 + 512].partition_broadcast(128), "mb", [], [mb])
                for q in range(4):
                    mw = mws[mcnt % 2]
                    self.dma("sp", mw[:, :, :], mwv[:, :, c0 + q * 128:c0 + (q + 1) * 128], "mw%d" % (mcnt % 2), [], [mw])
                    mcnt += 1
                    for kc in range(8):
                        self.mm(pst, pst[:, q * 128:(q + 1) * 128], self.condrep[s][:, kc, :], mw[:, kc, :],
                                kc == 0, kc == 7, [self.condrep[s], mw])
                self.tt("dve", dest[:, hf * 512:(hf + 1) * 512], pst[:, :], mb[:, :], ALU.add, [pst, mb], [dest])

    def bcast_row(self, dest, dram_row, sem="bc"):
        self.dma("sp", dest[:, :], dram_row.partition_broadcast(128), sem, [], [dest])

    def norm_mod_T(self, xt, M, Sh, hT_ps, hT, tmp, hb, ss, col=None):
        self.act(tmp[:, :], xt[:, :], AF.Square, [xt], [tmp, ss], accum_out=ss[:, 0:1])
        self.act(ss[:, 1:2], ss[:, 0:1], AF.Sqrt, [ss], [ss], bias=self.epsb[:, 0:1], scale=1.0 / D)
        self.S.op("dve", lambda e: e.reciprocal(out=ss[:, 2:3], in_=ss[:, 1:2]), reads=[ss.b], writes=[ss.b])
        self.stt(tmp[:, :], xt[:, :], ss[:, 2:3], M[:, :], ALU.mult, ALU.mult, [xt, ss, M], [tmp])
        self.tt("pool", hb[:, :], tmp[:, :], Sh[:, :], ALU.add, [tmp, Sh], [hb])
        for kc in range(8):
            self.tr(hT_ps, hT_ps[:, kc * 128:(kc + 1) * 128], hb[:, kc * 128:(kc + 1) * 128], self.identb[:, :],
                    [hb, self.identb])
        hv = hT_ps[:, :].rearrange("p (k t) -> p k t", k=8)
        if col is None:
            self.cp("act", hT[:, :, :], hv, [hT_ps], [hT])
        else:
            self.cp("act", hT[:, :, col], hv, [hT_ps], [hT])

    def phase3(self, l, dst, bdst):
        S = self.S
        NT = self.NT
        with ExitStack() as es:
            w1b = self.sb(es, [128, 8, DFF], BF16, "w1b")
            w2b = self.sb(es, [128, 32, D], BF16, "w2b")
            w1v = self.w1[l].rearrange("(kc p) n -> p kc n", p=128)
            w2v = self.w2[l].rearrange("(fc p) n -> p fc n", p=128)
            for kc in range(8):
                for q in range(4):
                    self.dma("pool", w1b[:, kc, q * 1024:(q + 1) * 1024], w1v[:, kc, q * 1024:(q + 1) * 1024],
                             "w1", [], [w1b])
            for fc in range(0, 32, 4):
                self.dma("pool", w2b[:, fc:fc + 4, :], w2v[:, fc:fc + 4, :], "w2", [], [w2b])
            self.epsb = self.sb(es, [128, 1], F32, "epsb")
            self.memset("dve", self.epsb[:, :], EPS, [self.epsb])
            M2 = self.sb(es, [128, D], F32, "M2")
            Sh2 = self.sb(es, [128, D], F32, "Sh2")
            G2 = self.sb(es, [128, D], F32, "G2")
            pb = [self.ps(es, [128, 512], F32, "pb%d" % i) for i in range(4)]
            po = [self.ps(es, [128, 1024], F32, "po%d" % i) for i in range(1)]
            hT_ps = self.ps(es, [128, 1024], BF16, "hTps")
            xts = [self.sb(es, [128, D], F32, "xt%d" % i) for i in range(2)]
            tmp = self.sb(es, [128, D], F32, "tmp")
            hb = self.sb(es, [128, D], BF16, "hb")
            ss = self.sb(es, [128, 4], F32, "ss")
            h2T = self.sb(es, [128, 8, 256], BF16, "h2T")
            uT = self.sb(es, [128, 32, 256], BF16, "uT")
            rl = [self.sb(es, [128, 512], F32, "rl%d" % i) for i in range(2)]
            for s in range(self.nseq):
                with ExitStack() as es2:
                    self.compute_mod(es2, l, s, 3072, [Sh2, M2, G2], pb)
                    self.bcast_row(tmp, self.g_mlp_pre[l])
                    self.stt(M2[:, :], M2[:, :], 1.0, tmp[:, :], ALU.add, ALU.mult, [M2, tmp], [M2])
                    self.bcast_row(tmp, self.g_mlp_post[l])
                    self.tt("dve", G2[:, :], G2[:, :], tmp[:, :], ALU.mult, [G2, tmp], [G2])
                    self.S.barrier()
                for g in range(NT // 2):
                    xg = [xts[j] for j in range(2)]
                    for j in range(2):
                        t0 = (2 * g + j) * 128
                        self.dma("sp", xg[j][:, :], self.X1[s, t0:t0 + 128, :], "xt%d" % j,
                                 [self.bX1[s]], [xg[j]])
                        self.norm_mod_T(xg[j], M2, Sh2, hT_ps, h2T, tmp, hb, ss, col=slice(j * 128, (j + 1) * 128))
                    for fp in range(16):
                        pst = pb[fp % 4]
                        for sub in range(2):
                            fc = 2 * fp + sub
                            for kc in range(8):
                                self.mm(pst, pst[:, sub * 256:(sub + 1) * 256], w1b[:, kc, fc * 128:(fc + 1) * 128],
                                        h2T[:, kc, :], kc == 0, kc == 7, [w1b, h2T])
                        r = rl[fp % 2]
                        self.act(r[:, :], pst[:, :], AF.Relu, [pst], [r])
                        self.tt("pool", uT[:, 2 * fp:2 * fp + 2, :], r[:, :].rearrange("p (a b) -> p a b", a=2),
                                r[:, :].rearrange("p (a b) -> p a b", a=2), ALU.mult, [r], [uT])
                    for j in range(2):
                        t0 = (2 * g + j) * 128
                        pot = po[0]
                        for hf in range(2):
                            for fc in range(32):
                                self.mm(pot, pot[:, hf * 512:(hf + 1) * 512], uT[:, fc, j * 128:(j + 1) * 128],
                                        w2b[:, fc, hf * 512:(hf + 1) * 512], fc == 0, fc == 31, [uT, w2b])
                        self.act(tmp[:, :], pot[:, :], AF.Square, [pot], [tmp, ss], accum_out=ss[:, 0:1])
                        self.act(ss[:, 1:2], ss[:, 0:1], AF.Sqrt, [ss], [ss], bias=self.epsb[:, 0:1], scale=1.0 / D)
                        S.op("dve", lambda e: e.reciprocal(out=ss[:, 2:3], in_=ss[:, 1:2]), reads=[ss.b], writes=[ss.b])
                        self.stt(tmp[:, :], pot[:, :], ss[:, 2:3], G2[:, :], ALU.mult, ALU.mult, [pot, ss, G2], [tmp])
                        o = xg[j]
                        self.tt("pool", o[:, :], tmp[:, :], xg[j][:, :], ALU.add, [tmp, xg[j]], [o])
                        self.dma("sp", dst[s, t0:t0 + 128, :], o[:, :], "xt%d" % j, [o], [bdst[s]])
            S.barrier()


    def ctab(self, es, name, dt=F32, shape=None):
        o, n = _off[name]
        shp = [128, n] if shape is None else shape
        t = self.sb(es, shp, dt, "c" + name)
        src = self.consts[:, o:o + n]
        dst = t[:, :] if shape is None else t.t
        if shape is not None:
            letters = "abc"[:len(shape) - 1]
            pat = "p (" + " ".join(letters) + ") -> p " + " ".join(letters)
            kw = {letters[k]: shape[k + 1] for k in range(len(letters) - 1)}
            src = src.rearrange(pat, **kw)
            dst = t[tuple([slice(None)] * len(shape))]
        if dt == F32:
            self.dma("sp", dst, src, "ct", [], [t])
        else:
            self.dma("pool", dst, src, "ctp", [], [t])
        return t

    def rope_tables(self, es, s, invT, nf, name):
        NT = self.NT
        TWO_PI = 2.0 * math.pi
        C1 = 6.28125
        C2 = TWO_PI - C1
        PI_SAFE = 3.1415925
        outs = []
        r_tiles = [self.sb(es, [128, NT, nf], F32, name + nm) for nm in ("sin", "cos")]
        with ExitStack() as es2:
            ang = self.sb(es2, [128, NT, nf], F32, name + "ang")
            u = self.sb(es2, [128, NT, nf], F32, name + "u")
            ki = self.sb(es2, [128, NT, nf], I32, name + "ki")
            for i in range(NT):
                c = s * NT + i
                self.ts("pool", ang[:, i, :], invT[:, :], self.posf[:, c:c + 1], None, ALU.mult, None,
                        [invT, self.posf], [ang])
            for r, shift in zip(r_tiles, (0.0, 0.25)):
                self.ts("dve", u[:, :, :], ang[:, :, :], 1.0 / TWO_PI, shift, ALU.mult, ALU.add, [ang], [u])
                self.cp("dve", ki[:, :, :], u[:, :, :], [u], [ki])
                self.cp("dve", u[:, :, :], ki[:, :, :], [ki], [u])
                self.stt(r[:, :, :], u[:, :, :], -C1, ang[:, :, :], ALU.mult, ALU.add, [u, ang], [r])
                self.stt(r[:, :, :], u[:, :, :], -C2, r[:, :, :], ALU.mult, ALU.add, [u, r], [r])
                self.ts("dve", r[:, :, :], r[:, :, :], shift * TWO_PI, PI_SAFE, ALU.add, ALU.min, [r], [r])
                self.ts("dve", r[:, :, :], r[:, :, :], -PI_SAFE, None, ALU.max, None, [r], [r])
                self.act(r[:, :, :], r[:, :, :], AF.Sin, [r], [r])
            self.S.barrier()
        return r_tiles[1], r_tiles[0]

    def rope(self, src_t, src, H, Dh, half, cos_i, sin_i, dst_t, dst, tmps):
        t1, t2, t3, t4 = tmps
        x1 = src[:, :, 0:half]
        x2 = src[:, :, half:2 * half]
        cb = cos_i.unsqueeze(1).to_broadcast([128, H, half])
        sbn = sin_i.unsqueeze(1).to_broadcast([128, H, half])
        v = lambda t: t[:, 0:H * half].rearrange("p (h d) -> p h d", h=H)
        rd = [src_t, self.cosT, self.sinT]
        self.tt("dve", v(t1), x1, cb, ALU.mult, rd, [t1])
        self.tt("pool", v(t2), x2, sbn, ALU.mult, rd, [t2])
        self.tt("dve", dst[:, :, 0:half], v(t1), v(t2), ALU.subtract, [t1, t2], [dst_t])
        self.tt("pool", v(t3), x1, sbn, ALU.mult, rd, [t3])
        self.tt("dve", v(t4), x2, cb, ALU.mult, rd, [t4])
        self.tt("pool", dst[:, :, half:2 * half], v(t3), v(t4), ALU.add, [t3, t4], [dst_t])
        if 2 * half < Dh:
            self.cp("act", dst[:, :, 2 * half:Dh], src[:, :, 2 * half:Dh], [src_t], [dst_t])

    def head_norm(self, Y, H, center, g_view, out_t, out_view, sq, st, gate_view=None, gate_t=None):
        Yv = Y[:, 0:H * 64].rearrange("p (h d) -> p h d", h=H)
        sqv = sq[:, 0:H * 64].rearrange("p (h d) -> p h d", h=H)
        if center:
            self.S.op("dve", lambda e: e.tensor_reduce(out=st[:, 0:H], in_=Yv, axis=AX.X, op=ALU.add),
                      reads=[Y.b], writes=[st.b])
            self.ts("dve", st[:, 0:H], st[:, 0:H], 1.0 / 64, None, ALU.mult, None, [st], [st])
            self.tt("dve", Yv, Yv, st[:, 0:H].unsqueeze(2).to_broadcast([128, H, 64]), ALU.subtract, [Y, st], [Y])
        self.tt("pool", sqv, Yv, Yv, ALU.mult, [Y], [sq])
        self.S.op("dve", lambda e: e.tensor_reduce(out=st[:, 16:16 + H], in_=sqv, axis=AX.X, op=ALU.add),
                  reads=[sq.b], writes=[st.b])
        self.act(st[:, 32:32 + H], st[:, 16:16 + H], AF.Sqrt, [st], [st], bias=self.epsb[:, 0:1], scale=1.0 / 64)
        self.S.op("dve", lambda e: e.reciprocal(out=st[:, 48:48 + H], in_=st[:, 32:32 + H]), reads=[st.b], writes=[st.b])
        self.tt("dve", Yv, Yv, st[:, 48:48 + H].unsqueeze(2).to_broadcast([128, H, 64]), ALU.mult, [Y, st], [Y])
        if gate_view is None:
            self.tt("pool", out_view, Yv, g_view, ALU.mult, [Y, self.hg], [out_t])
        else:
            self.tt("pool", Yv, Yv, g_view, ALU.mult, [Y, self.hg], [Y])
            self.tt("pool", out_view, Yv, gate_view, ALU.mult, [Y, gate_t], [out_t])

    def rms_resid(self, pot, G, xt, tmp, ss):
        self.act(tmp[:, :], pot[:, :], AF.Square, [pot], [tmp, ss], accum_out=ss[:, 0:1])
        self.act(ss[:, 1:2], ss[:, 0:1], AF.Sqrt, [ss], [ss], bias=self.epsb[:, 0:1], scale=1.0 / D)
        self.S.op("dve", lambda e: e.reciprocal(out=ss[:, 2:3], in_=ss[:, 1:2]), reads=[ss.b], writes=[ss.b])
        self.stt(tmp[:, :], pot[:, :], ss[:, 2:3], G[:, :], ALU.mult, ALU.mult, [pot, ss, G], [tmp])
        self.tt("pool", xt[:, :], tmp[:, :], xt[:, :], ALU.add, [tmp, xt], [xt])

    def load_mods1(self, l, s, FB, Sh1, M1, G1, tmp):
        with ExitStack() as es2:
            self.compute_mod(es2, l, s, 0, [Sh1, M1], FB)
            if G1 is not None:
                self.compute_mod(es2, l, s, 2048, [G1], FB)
            self.bcast_row(tmp, self.g_attn_pre[l])
            self.stt(M1[:, :], M1[:, :], 1.0, tmp[:, :], ALU.add, ALU.mult, [M1, tmp], [M1])
            if G1 is not None:
                self.bcast_row(tmp, self.g_attn_post[l])
                self.tt("dve", G1[:, :], G1[:, :], tmp[:, :], ALU.mult, [G1, tmp], [G1])
            self.S.barrier()

    def phase2(self, l, src, bsrc):
        S = self.S
        NT = self.NT
        with ExitStack() as es:
            wBC = self.sb(es, [128, 8, 1808], BF16, "wBC")
            woutb = self.sb(es, [128, 8, D], BF16, "woutb")
            wv = self.w_in[l].rearrange("(kc p) n -> p kc n", p=128)
            wov = self.w_out[l].rearrange("(kc p) n -> p kc n", p=128)
            for kc in range(8):
                self.dma("pool", wBC[:, kc, :], wv[:, kc, 968:2776], "wBC", [], [wBC])
                self.dma("pool", woutb[:, kc, :], wov[:, kc, :], "wout", [], [woutb])
            self.epsb = self.sb(es, [128, 1], F32, "epsb")
            self.memset("dve", self.epsb[:, :], EPS, [self.epsb])
            RMASK = self.ctab(es, "RMASK")
            RXI = self.ctab(es, "RXI")
            RZ = self.ctab(es, "RZ")
            RG = self.ctab(es, "RG")
            TRI = self.ctab(es, "TRI")
            SU = self.ctab(es, "SU")
            LM4 = self.ctab(es, "LM4")
            UM4 = self.ctab(es, "UM4")
            INVB = self.ctab(es, "INVB")
            self.hg = self.sb(es, [128, 512], F32, "hg")
            self.bcast_row(self.hg, self.head_g[l, 256:768])
            wa2 = self.sb(es, [128, 128], F32, "wa2")
            self.memset("dve", wa2[:, :], 0.0, [wa2])
            self.dma("sp", wa2[0:16, :], self.gla_wa2[l], "ct", [], [wa2])
            self.dma("sp", wa2[16:17, :], self.gla_ba[l:l + 1, :], "ct", [], [wa2])
            alT = self.sb(es, [128, 128], F32, "alT")
            alp = self.sb(es, [128, 128], F32, "alp")
            self.memset("dve", alp[:, :], 0.0, [alp])
            self.memset("dve", alp[:, 16:17], 1.0, [alp])
            M1 = self.sb(es, [128, D], F32, "M1")
            Sh1 = self.sb(es, [128, D], F32, "Sh1")
            G1 = self.sb(es, [128, D], F32, "G1")
            R2 = self.sb(es, [128, 256], F32, "R2")
            Rbf = self.sb(es, [128, 256], BF16, "Rbf")
            S2 = self.sb(es, [128, 256], F32, "S2")
            Sbf = self.sb(es, [128, 256], BF16, "Sbf")
            FB = [self.ps(es, [128, 512], F32, "FB%d" % i) for i in range(4)]
            TB = [self.ps(es, [128, 1024], BF16, "TB%d" % i) for i in range(1)]
            PW = self.ps(es, [128, 1024], F32, "PW")
            xt = self.sb(es, [128, D], F32, "xt")
            tmp = self.sb(es, [128, D], F32, "tmp")
            hb = self.sb(es, [128, D], BF16, "hb")
            ss = self.sb(es, [128, 4], F32, "ss")
            hT = self.sb(es, [128, 8, 128], BF16, "hT")
            pj = self.sb(es, [128, 1808], F32, "pj")
            pjs2 = [pj, self.sb(es, [128, 1808], F32, "pjB")]
            xts2 = [xt, self.sb(es, [128, D], F32, "xtB")]
            tmpA = self.sb(es, [128, D], F32, "tmpA")
            ssA = self.sb(es, [128, 4], F32, "ssA")
            RB = self.sb(es, [128, 512], BF16, "RB")
            rt = [self.sb(es, [128, 256], F32, "rt%d" % k) for k in range(4)]
            kz = self.sb(es, [128, 256], BF16, "kz")
            vB = self.sb(es, [128, 256], BF16, "vB")
            gBC = self.sb(es, [128, 512], F32, "gBC")
            qT2 = self.sb(es, [128, 4, 128], BF16, "qT2")
            qxT2 = self.sb(es, [128, 4, 128], BF16, "qxT2")
            kT2 = self.sb(es, [128, 2, 128], BF16, "kT2")
            HMQ = self.ctab(es, "HMQ")
            BD1 = self.ctab(es, "BD1")
            BDS = self.ctab(es, "BDS")
            qpbd = self.sb(es, [128, 4, 128], BF16, "qpbd")
            qmbd = self.sb(es, [128, 4, 128], BF16, "qmbd")
            sTm = self.sb(es, [128, 4, 128], BF16, "sTm")
            Y = self.sb(es, [128, 512], F32, "Y")
            sq = self.sb(es, [128, 512], F32, "sq")
            st = self.sb(es, [128, 64], F32, "st")
            al = self.sb(es, [128, 16], F32, "al")
            ez = self.sb(es, [128, 128], F32, "ez")
            SP = self.sb(es, [128, 128], F32, "SP")
            Ep = self.sb(es, [128, 128], F32, "Ep")
            Em = self.sb(es, [128, 128], F32, "Em")
            Ez = self.sb(es, [128, 128], F32, "Ez")
            CQK = self.sb(es, [128, 256], BF16, "CQK")
            kzC = self.sb(es, [128, 128], BF16, "kzC")
            qpT = self.sb(es, [128, 128], BF16, "qpT")
            qmT = self.sb(es, [128, 128], BF16, "qmT")
            kpT = self.sb(es, [128, 128], BF16, "kpT")
            kmT = self.sb(es, [128, 128], BF16, "kmT")
            a1 = self.sb(es, [128, 512], F32, "a1")
            a2 = self.sb(es, [128, 512], F32, "a2")
            AT = self.sb(es, [128, 4, 128], BF16, "AT")
            vC = self.sb(es, [128, 256], BF16, "vC")
            yt = self.sb(es, [128, D], BF16, "yt")
            yT = self.sb(es, [128, 8, 128], BF16, "yT")
            fbi = [0]

            def fb():
                fbi[0] += 1
                return FB[fbi[0] % 4]

            for s in range(self.nseq):
                with ExitStack() as es_s:
                    self.load_mods1(l, s, FB, Sh1, M1, G1, tmp)
                    self.cosT, self.sinT = self.rope_tables(es_s, s, INVB, 32, "rB")
                    cosT, sinT = self.cosT, self.sinT
                    for z_ in (R2, S2):
                        self.memset("dve", z_[:, :], 0.0, [z_])
                    for z_ in (Rbf, Sbf):
                        self.memset("pool", z_[:, :], 0.0, [z_])
                    def load_x(i_):
                        par = i_ % 2
                        self.dma("sp", xts2[par][:, :], src[s, i_ * 128:i_ * 128 + 128, :], "xt%d" % par, [bsrc[s]], [xts2[par]])

                    def partA2(i_):
                        par = i_ % 2
                        xt_, pj_ = xts2[par], pjs2[par]
                        self.act(tmpA[:, :], xt_[:, :], AF.Square, [xt_], [tmpA, ssA], accum_out=ssA[:, 0:1])
                        self.act(ssA[:, 1:2], ssA[:, 0:1], AF.Ln, [ssA], [ssA], bias=self.epsb[:, 0:1], scale=1.0 / D)
                        self.act(ssA[:, 2:3], ssA[:, 1:2], AF.Exp, [ssA], [ssA], scale=-0.5)
                        self.act(tmpA[:, :], xt_[:, :], AF.Copy, [xt_, ssA], [tmpA], scale=ssA[:, 2:3])
                        self.tt("pool", tmpA[:, :], tmpA[:, :], M1[:, :], ALU.mult, [tmpA, M1], [tmpA])
                        self.tt("pool", hb[:, :], tmpA[:, :], Sh1[:, :], ALU.add, [tmpA, Sh1], [hb])
                        tbp = TB[0]
                        for kc in range(8):
                            self.tr(tbp, tbp[:, kc * 128:(kc + 1) * 128], hb[:, kc * 128:(kc + 1) * 128], self.identb[:, :],
                                    [hb, self.identb])
                        self.cp("act", hT[:, :, :], tbp[:, :].rearrange("p (k t) -> p k t", k=8), [tbp], [hT])
                        for j, (c0, c1) in enumerate(((0, 512), (512, 1024), (1024, 1536), (1536, 1808))):
                            f = fb()
                            for kc in range(8):
                                self.mm(f, f[:, 0:c1 - c0], hT[:, kc, :], wBC[:, kc, c0:c1], kc == 0, kc == 7, [hT, wBC])
                            self.cp("act", pj_[:, c0:c1], f[:, 0:c1 - c0], [f], [pj_])

                    load_x(0)
                    partA2(0)
                    for i in range(NT):
                        t0 = i * 128
                        xt = xts2[i % 2]
                        pj = pjs2[i % 2]
                        if i + 1 < NT:
                            load_x(i + 1)
                        self.dma("sp", yt[:, 0:256], self.YAD[s, t0:t0 + 128, 0:256], "yt", [self.bYAD[s]], [yt])
                        self.dma("sp", yt[:, 768:1024], self.YAD[s, t0:t0 + 128, 256:512], "yt", [self.bYAD[s]], [yt])
                        self.rope(pj, pj[:, 0:512].rearrange("p (h d) -> p h d", h=8), 8, 64, 32,
                                  cosT[:, i, :], sinT[:, i, :], RB, RB[:, :].rearrange("p (h d) -> p h d", h=8), rt)
                        self.tt("pool", kz[:, :], RB[:, 256:512], RZ[:, :], ALU.mult, [RB, RZ], [kz])
                        self.cp("act", vB[:, :], pj[:, 512:768], [pj], [vB])
                        self.act(gBC[:, 0:256], pj[:, 768:1024], AF.Silu, [pj], [gBC])
                        self.act(gBC[:, 256:512], pj[:, 1552:1808], AF.Silu, [pj], [gBC])
                        tb = TB[0]
                        for k in range(4):
                            self.tr(tb, tb[:, k * 128:(k + 1) * 128], RB[:, k * 128:(k + 1) * 128], self.identb[:, :], [RB, self.identb])
                        self.tt("dve", qT2[:, :, :].rearrange("p (a b) t -> p a b t", a=2),
                                tb[:, 0:256].rearrange("p (a t) -> p a t", a=2).unsqueeze(2).to_broadcast([128, 2, 2, 128]),
                                HMQ[:, :].rearrange("p (b t) -> p b t", b=2).unsqueeze(1).to_broadcast([128, 2, 2, 128]),
                                ALU.mult, [tb, HMQ], [qT2])
                        self.cp("dve", kT2[:, :, :], tb[:, 256:512].rearrange("p (a t) -> p a t", a=2), [tb], [kT2])
                        self.tt("pool", qxT2[:, :, :], qT2[:, :, :], RXI[:, :].rearrange("p (a t) -> p a t", a=4), ALU.mult,
                                [qT2, RXI], [qxT2])
                        f = fb()
                        for h in range(4):
                            self.mm(f, f[:, h * 128:(h + 1) * 128], kT2[:, h // 2, :], qT2[:, h, :], True, True, [kT2, qT2])
                        self.tt("dve", sTm[:, :, :], f[:, :].rearrange("p (h t) -> p h t", h=4),
                                RMASK[:, :].rearrange("p (h t) -> p h t", h=4), ALU.mult, [f, RMASK], [sTm])
                        fo = fb()
                        first = True
                        for h in range(4):
                            a, b = h // 2, h % 2
                            self.mm(fo, fo[:, h * 64:(h + 1) * 64], sTm[:, h, :], vB[:, h * 64:(h + 1) * 64], first, False, [sTm, vB])
                            first = False
                            self.mm(fo, fo[:, h * 64:(h + 1) * 64], qxT2[:, h, :], Rbf[:, a * 128 + 64 * b:a * 128 + 64 * b + 64],
                                    False, h == 3, [qxT2, Rbf])
                        self.cp("act", Y[:, 0:256], fo[:, 0:256], [fo], [Y])
                        fu = fb()
                        for a in range(2):
                            self.mm(fu, fu[:, a * 128:(a + 1) * 128], kz[:, a * 128:(a + 1) * 128], vB[:, a * 128:(a + 1) * 128],
                                    True, True, [kz, vB])
                        self.tt("dve", R2[:, :], R2[:, :], RG[:, :], ALU.mult, [R2, RG], [R2])
                        self.tt("dve", R2[:, :], R2[:, :], fu[:, 0:256], ALU.add, [R2, fu], [R2])
                        self.cp("act", Rbf[:, :], R2[:, :], [R2], [Rbf])
                        if i + 1 < NT:
                            partA2(i + 1)
                        self.cp("act", alp[:, 0:16], pj[:, 1536:1552], [pj], [alp])
                        fz = fb()
                        self.S.op("pe", lambda e: e.transpose(fz[:, 0:128], alp[:, :], self.identf[:, :]),
                                  reads=[alp.b, self.identf.b], writes=[fz.b])
                        self.cp("act", alT[:, :], fz[:, 0:128], [fz], [alT])
                        fz2 = fb()
                        self.mm(fz2, fz2[:, 0:128], alT[:, :], wa2[:, :], True, True, [alT, wa2])
                        self.act(ez[:, :], fz2[:, 0:128], AF.Exp, [fz2], [ez], scale=-1.0)
                        self.act(SP[:, :], ez[:, :], AF.Ln, [ez], [SP], bias=self.oneb[:, 0:1])
                        fbt = fb()
                        self.mm(fbt, fbt[:, 0:128], SP[:, :], TRI[:, :], True, True, [SP, TRI])
                        self.mm(fbt, fbt[:, 128:256], SU[:, :], SP[:, :], True, True, [SU, SP])
                        self.act(Ep[:, :], fbt[:, 0:128], AF.Exp, [fbt], [Ep])
                        self.act(Em[:, :], fbt[:, 0:128], AF.Exp, [fbt], [Em], scale=-1.0)
                        self.act(Ez[:, :], fbt[:, 128:256], AF.Exp, [fbt], [Ez])
                        self.cp("act", CQK[:, :], pj[:, 1024:1280], [pj], [CQK])
                        self.tt("pool", kzC[:, :], pj[:, 1152:1280], Ez[:, :], ALU.mult, [pj, Ez], [kzC])
                        self.cp("act", vC[:, :], pj[:, 1280:1536], [pj], [vC])
                        tb = TB[0]
                        self.tr(tb, tb[:, 0:128], CQK[:, 0:128], self.identb[:, :], [CQK, self.identb])
                        self.tr(tb, tb[:, 128:256], CQK[:, 128:256], self.identb[:, :], [CQK, self.identb])
                        qs = 32.0 ** -0.5
                        self.stt(qpT[:, :], tb[:, 0:128], qs, Ep[:, :], ALU.mult, ALU.mult, [tb, Ep], [qpT])
                        self.stt(qmT[:, :], tb[:, 0:128], qs, Em[:, :], ALU.mult, ALU.mult, [tb, Em], [qmT])
                        self.tt("dve", kpT[:, :], tb[:, 128:256], Ep[:, :], ALU.mult, [tb, Ep], [kpT])
                        self.tt("dve", kmT[:, :], tb[:, 128:256], Em[:, :], ALU.mult, [tb, Em], [kmT])
                        self.tt("pool", qpbd[:, :, :], qpT[:, :].unsqueeze(1).to_broadcast([128, 4, 128]),
                                BD1[:, :].rearrange("p (h t) -> p h t", h=4), ALU.mult, [qpT, BD1], [qpbd])
                        self.tt("pool", qmbd[:, :, :], qmT[:, :].unsqueeze(1).to_broadcast([128, 4, 128]),
                                BD1[:, :].rearrange("p (h t) -> p h t", h=4), ALU.mult, [qmT, BD1], [qmbd])
                        fa1 = fb()
                        fa2 = fb()
                        self.mm(fa1, fa1[:, :], kmT[:, :], qpbd[:, :, :], True, True, [kmT, qpbd])
                        self.mm(fa2, fa2[:, :], kpT[:, :], qmbd[:, :, :], True, True, [kpT, qmbd])
                        self.tt("dve", a1[:, :], fa1[:, :], LM4[:, :], ALU.mult, [fa1, LM4], [a1])
                        self.tt("dve", a2[:, :], fa2[:, :], UM4[:, :], ALU.mult, [fa2, UM4], [a2])
                        self.tt("pool", AT[:, :, :], a1[:, :].rearrange("p (h t) -> p h t", h=4),
                                a2[:, :].rearrange("p (h t) -> p h t", h=4), ALU.add, [a1, a2], [AT])
                        foc = fb()
                        for h in range(4):
                            self.mm(foc, foc[:, h * 64:(h + 1) * 64], AT[:, h, :], vC[:, h * 64:(h + 1) * 64], h == 0, False, [AT, vC])
                        self.mm(foc, foc[:, 0:256], qpT[:, :], Sbf[:, :], False, True, [qpT, Sbf])
                        self.cp("act", Y[:, 256:512], foc[:, 0:256], [foc], [Y])
                        fuc = fb()
                        self.mm(fuc, fuc[:, 0:256], kzC[:, :], vC[:, :], True, True, [kzC, vC])
                        self.stt(S2[:, :], S2[:, :], Ep[:, 127:128], fuc[:, 0:256], ALU.mult, ALU.add, [S2, Ep, fuc], [S2])
                        self.tt("pool", Sbf[:, :], S2[:, :], BDS[:, :], ALU.mult, [S2, BDS], [Sbf])
                        self.head_norm(Y, 8, True, self.hg[:, :].rearrange("p (h d) -> p h d", h=8), yt,
                                       yt[:, 256:768].rearrange("p (h d) -> p h d", h=8), sq, st,
                                       gate_view=gBC[:, :].rearrange("p (h d) -> p h d", h=8), gate_t=gBC)
                        tb = TB[0]
                        for kc in range(8):
                            self.tr(tb, tb[:, kc * 128:(kc + 1) * 128], yt[:, kc * 128:(kc + 1) * 128], self.identb[:, :],
                                    [yt, self.identb])
                        self.cp("act", yT[:, :, :], tb[:, :].rearrange("p (k t) -> p k t", k=8), [tb], [yT])
                        for hf in range(2):
                            for kc in range(8):
                                self.mm(PW, PW[:, hf * 512:(hf + 1) * 512], yT[:, kc, :], woutb[:, kc, hf * 512:(hf + 1) * 512],
                                        kc == 0, kc == 7, [yT, woutb])
                        self.rms_resid(PW, G1, xt, tmp, ss)
                        self.dma("sp", self.X1[s, t0:t0 + 128, :], xt[:, :], "xt%d" % (i % 2), [xt], [self.bX1[s]])
                    S.barrier()
            S.barrier()


    def phase1(self, l, src, bsrc):
        S = self.S
        NT = self.NT
        T_ = self.T
        lam_init = 0.8 - 0.6 * math.exp(-0.3 * l)
        with ExitStack() as es:
            wAD = self.sb(es, [128, 8, 1792], BF16, "wAD")
            wv = self.w_in[l].rearrange("(kc p) n -> p kc n", p=128)
            for kc in range(8):
                for (d0, s0, n) in ((0, 384, 512), (512, 0, 320), (832, 896, 64), (896, 320, 64), (960, 960, 8),
                                    (1024, 2776, 768)):
                    self.dma("pool", wAD[:, kc, d0:d0 + n], wv[:, kc, s0:s0 + n], "wAD", [], [wAD])
            for kc in range(8):
                self.memset("dve", wAD[:, kc, 968:1024], 0.0, [wAD])
            self.epsb = self.sb(es, [128, 1], F32, "epsb")
            self.memset("dve", self.epsb[:, :], EPS, [self.epsb])
            INVA = self.ctab(es, "INVA")
            INVD = self.ctab(es, "INVD")
            DMQb = self.ctab(es, "DMQ", BF16)
            DMQ30 = self.ctab(es, "DMQ30")
            P2H2 = self.ctab(es, "P2H2")
            P2HN = self.ctab(es, "P2HN")
            I4 = self.sb(es, [128, 4, 128], BF16, "I4")
            for k in range(4):
                self.cp("dve", I4[:, k, :], self.identb[:, :], [self.identb], [I4])
            self.hg = self.sb(es, [128, 512], F32, "hg")
            self.dma("sp", self.hg[:, 0:256], self.head_g[l, 0:256].partition_broadcast(128), "hg", [], [self.hg])
            self.dma("sp", self.hg[:, 256:512], self.head_g[l, 768:1024].partition_broadcast(128), "hg", [], [self.hg])
            self.ts("dve", self.hg[:, 256:512], self.hg[:, 256:512], 1.0 - lam_init, None, ALU.mult, None, [self.hg], [self.hg])
            lamt = self.sb(es, [128, 4, 32], F32, "lamt")
            self.dma("sp", lamt[:, :, :], self.lam[l].partition_broadcast(128), "lamt", [], [lamt])
            lp = self.sb(es, [128, 2, 32], F32, "lp")
            lsm = self.sb(es, [128, 4], F32, "lsm")
            self.tt("dve", lp[:, :, :], lamt[:, 0:4:2, :], lamt[:, 1:4:2, :], ALU.mult, [lamt], [lp])
            S.op("dve", lambda e: e.tensor_reduce(out=lsm[:, 0:2], in_=lp[:, :, :], axis=AX.X, op=ALU.add),
                 reads=[lp.b], writes=[lsm.b])
            self.act(lsm[:, 2:4], lsm[:, 0:2], AF.Exp, [lsm], [lsm])
            nlam = self.sb(es, [128, 1], F32, "nlam")
            self.tt("dve", nlam[:, :], lsm[:, 3:4], lsm[:, 2:3], ALU.subtract, [lsm], [nlam])
            self.ts("dve", nlam[:, :], nlam[:, :], -lam_init, None, ALU.add, None, [nlam], [nlam])
            M1 = self.sb(es, [128, D], F32, "M1")
            Sh1 = self.sb(es, [128, D], F32, "Sh1")
            kiT2 = self.sb(es, [128, T_], BF16, "kiT2")
            akT2 = self.sb(es, [128, T_], BF16, "akT2")
            av = self.sb(es, [128, NT, 65], BF16, "av")
            dkT2 = self.sb(es, [128, 2, T_], BF16, "dkT2")
            dv = self.sb(es, [128, NT, 4, 65], BF16, "dv")
            score = self.sb(es, [128, T_], F32, "score")
            nm = self.sb(es, [128, T_], BF16, "nm")
            rl = [self.sb(es, [128, 512], F32, "rl%d" % k) for k in range(2)]
            FB = [self.ps(es, [128, 512], F32, "FB%d" % i) for i in range(3)]
            TB = [self.ps(es, [128, 1024], BF16, "TB%d" % i) for i in range(1)]
            OA = self.ps(es, [128, 512], F32, "OA")
            OD = [self.ps(es, [128, 512], F32, "OD%d" % i) for i in range(2)]
            xt = self.sb(es, [128, D], F32, "xt")
            tmp = self.sb(es, [128, D], F32, "tmp")
            hb = self.sb(es, [128, D], BF16, "hb")
            ss = self.sb(es, [128, 4], F32, "ss")
            hT = self.sb(es, [128, 8, 128], BF16, "hT")
            pj = self.sb(es, [128, 1792], F32, "pj")
            TA = self.sb(es, [128, 896], BF16, "TA")
            TD = self.sb(es, [128, 512], BF16, "TD")
            TK = self.sb(es, [128, 256], BF16, "TK")
            rt = [self.sb(es, [128, 128], F32, "rt%d" % k) for k in range(4)]
            wiS = self.sb(es, [128, 8], F32, "wiS")
            qiT2 = self.sb(es, [128, 8, 128], BF16, "qiT2")
            aqT2 = self.sb(es, [128, 4, 128], BF16, "aqT2")
            dqbd = self.sb(es, [128, 2, 4, 128], BF16, "dqbd")
            BDM = self.ctab(es, "BDM")
            HM = self.ctab(es, "HM")
            HMQ = self.ctab(es, "HMQ")
            PT = [self.sb(es, [128, 4, 128], BF16, "PT%d" % k) for k in range(2)]
            PD = [self.sb(es, [128, 4, 128], BF16, "PD%d" % k) for k in range(4)]
            bs = self.sb(es, [128, 8], F32, "bs")
            h2 = self.sb(es, [128, NIT + 1], F32, "h2")
            hn = self.sb(es, [128, NIT + 1], F32, "hn")
            oa = self.sb(es, [128, 4, 65], F32, "oa")
            od = self.sb(es, [128, 8, 65], F32, "od")
            rr = self.sb(es, [128, 16], F32, "rr")
            Y = self.sb(es, [128, 512], F32, "Y")
            tq = self.sb(es, [128, 512], F32, "tq")
            sq = self.sb(es, [128, 512], F32, "sq")
            st = self.sb(es, [128, 64], F32, "st")
            yo = self.sb(es, [128, 512], BF16, "yo")
            fbi = [0]

            def fb():
                fbi[0] += 1
                return FB[fbi[0] % 3]

            xts = [xt, xt]
            tmps = [tmp, tmp]
            hbs = [hb, hb]
            hTs = [hT, hT]
            pjs = [pj, pj]
            sss = [ss, ss]

            def partA(s_, i_):
                par = i_ % 2
                xt_, tmp_, hb_, hT_, pj_, ss_ = xts[par], tmps[par], hbs[par], hTs[par], pjs[par], sss[par]
                t0_ = i_ * 128
                self.dma("sp", xt_[:, :], src[s_, t0_:t0_ + 128, :], "xt", [bsrc[s_]], [xt_])
                self.act(tmp_[:, :], xt_[:, :], AF.Square, [xt_], [tmp_, ss_], accum_out=ss_[:, 0:1])
                self.act(ss_[:, 1:2], ss_[:, 0:1], AF.Ln, [ss_], [ss_], bias=self.epsb[:, 0:1], scale=1.0 / D)
                self.act(ss_[:, 2:3], ss_[:, 1:2], AF.Exp, [ss_], [ss_], scale=-0.5)
                self.act(tmp_[:, :], xt_[:, :], AF.Copy, [xt_, ss_], [tmp_], scale=ss_[:, 2:3])
                self.tt("pool", tmp_[:, :], tmp_[:, :], M1[:, :], ALU.mult, [tmp_, M1], [tmp_])
                self.tt("pool", hb_[:, :], tmp_[:, :], Sh1[:, :], ALU.add, [tmp_, Sh1], [hb_])
                tbp = TB[0]
                for kc in range(8):
                    self.tr(tbp, tbp[:, kc * 128:(kc + 1) * 128], hb_[:, kc * 128:(kc + 1) * 128], self.identb[:, :],
                            [hb_, self.identb])
                self.cp("act", hT_[:, :, :], tbp[:, :].rearrange("p (k t) -> p k t", k=8), [tbp], [hT_])
                for j in range(4):
                    c0, c1 = j * 512, min((j + 1) * 512, 1792)
                    f = fb()
                    for kc in range(8):
                        self.mm(f, f[:, 0:c1 - c0], hT_[:, kc, :], wAD[:, kc, c0:c1], kc == 0, kc == 7, [hT_, wAD])
                    self.cp("act", pj_[:, c0:c1], f[:, 0:c1 - c0], [f], [pj_])

            for s in range(self.nseq):
                with ExitStack() as es_s:
                    self.load_mods1(l, s, FB, Sh1, M1, None, tmp)
                    cosA, sinA = self.rope_tables(es_s, s, INVA, 8, "rA")
                    cosD, sinD = self.rope_tables(es_s, s, INVD, 4, "rD")
                    self.memset("dve", av[:, :, :], 1.0, [av])
                    self.memset("pool", dv[:, :, :, :], 1.0, [dv])
                    for i in range(NT):
                        if self.cut(0):
                            break
                        t0 = i * 128
                        nk = t0 + 128
                        if i == 0:
                            partA(s, 0)
                        pj = pjs[i % 2]
                        self.cosT, self.sinT = cosA, sinA
                        self.rope(pj, pj[:, 0:896].rearrange("p (h d) -> p h d", h=14), 14, 64, 8,
                                  cosA[:, i, :], sinA[:, i, :], TA, TA[:, :].rearrange("p (h d) -> p h d", h=14), rt)
                        self.cosT, self.sinT = cosD, sinD
                        self.rope(pj, pj[:, 1024:1536].rearrange("p (h d) -> p h d", h=16), 16, 32, 4,
                                  cosD[:, i, :], sinD[:, i, :], TD, TD[:, :].rearrange("p (h d) -> p h d", h=16), rt)
                        self.cp("act", av[:, i, 0:64], pj[:, 896:960], [pj], [av])
                        self.cp("act", dv[:, i, :, 0:64], pj[:, 1536:1792].rearrange("p (h d) -> p h d", h=4), [pj], [dv])
                        self.cp("dve", wiS[:, :], pj[:, 960:968], [pj], [wiS])
                        if self.cut(2):
                            continue
                        self.cp("pool", TK[:, :].rearrange("p (a b d) -> p a b d", a=2, b=2),
                                TA[:, 768:896].rearrange("p (a d) -> p a d", a=2).unsqueeze(2).to_broadcast([128, 2, 2, 64]),
                                [TA], [TK])
                        if self.cut2(0):
                            continue
                        tb = TB[0]
                        for k in range(6):
                            self.tr(tb, tb[:, k * 128:(k + 1) * 128], TA[:, k * 128:(k + 1) * 128], self.identb[:, :], [TA, self.identb])
                        for k in range(2):
                            self.tr(tb, tb[:, (6 + k) * 128:(7 + k) * 128], TK[:, k * 128:(k + 1) * 128], self.identb[:, :], [TK, self.identb])
                        if self.cut2(1):
                            continue
                        hmb = HM[:, :].rearrange("p (b t) -> p b t", b=2)
                        self.tt("dve", qiT2[:, :, :].rearrange("p (a b) t -> p a b t", a=4),
                                tb[:, 0:512].rearrange("p (a t) -> p a t", a=4).unsqueeze(2).to_broadcast([128, 4, 2, 128]),
                                hmb.unsqueeze(1).to_broadcast([128, 4, 2, 128]), ALU.mult, [tb, HM], [qiT2])
                        if self.cut2(2):
                            continue
                        self.tt("dve", aqT2[:, :, :].rearrange("p (a b) t -> p a b t", a=2),
                                tb[:, 512:768].rearrange("p (a t) -> p a t", a=2).unsqueeze(2).to_broadcast([128, 2, 2, 128]),
                                HMQ[:, :].rearrange("p (b t) -> p b t", b=2).unsqueeze(1).to_broadcast([128, 2, 2, 128]),
                                ALU.mult, [tb, HMQ], [aqT2])
                        if self.cut2(3):
                            continue
                        self.cp("dve", akT2[:, t0:t0 + 128], tb[:, 768:896], [tb], [akT2])
                        self.cp("dve", kiT2[:, t0:t0 + 128], tb[:, 896:1024], [tb], [kiT2])
                        if self.cut2(4):
                            continue
                        tb = TB[0]
                        for k in range(4):
                            self.tr(tb, tb[:, k * 128:(k + 1) * 128], TD[:, k * 128:(k + 1) * 128], self.identb[:, :], [TD, self.identb])
                        if self.cut2(5):
                            continue
                        for g in range(2):
                            self.tt("dve", dqbd[:, g, :, :], tb[:, g * 128:(g + 1) * 128].unsqueeze(1).to_broadcast([128, 4, 128]),
                                    BDM[:, :].rearrange("p (h t) -> p h t", h=4), ALU.mult, [tb, BDM], [dqbd])
                        self.cp("dve", dkT2[:, :, t0:t0 + 128], tb[:, 256:512].rearrange("p (a t) -> p a t", a=2), [tb], [dkT2])
                        if self.cut(3):
                            continue
                        if i >= 2:
                            nb = (nk + 511) // 512
                            for h in range(8):
                                a, b = h // 2, h % 2
                                for kb in range(nb):
                                    w = min(512, nk - 512 * kb)
                                    f = fb()
                                    self.mm(f, f[:, 0:w], qiT2[:, h, :], kiT2[:, kb * 512:kb * 512 + w],
                                            True, True, [qiT2, kiT2])
                                    r = rl[(h * nb + kb) % 2]
                                    self.act(r[:, 0:w], f[:, 0:w], AF.Relu, [f], [r])
                                    sc = score[:, kb * 512:kb * 512 + w]
                                    if h == 0:
                                        self.ts("dve", sc, r[:, 0:w], wiS[:, 0:1], None, ALU.mult, None, [r, wiS], [score])
                                    else:
                                        self.stt(sc, r[:, 0:w], wiS[:, h:h + 1], sc, ALU.mult, ALU.add, [r, wiS, score], [score])
                            self.tt("dve", score[:, t0:nk], score[:, t0:nk], DMQ30[:, :], ALU.add, [score, DMQ30], [score])
                        for kt in range(i + 1):
                            fs = [fb(), fb()]
                            for g in range(2):
                                self.mm(fs[g], fs[g][:, :], dkT2[:, g, kt * 128:(kt + 1) * 128], dqbd[:, g, :, :], True, False,
                                        [dkT2, dqbd])
                                if kt == i:
                                    self.mm(fs[g], fs[g][:, :], DMQb[:, :], I4[:, :, :], False, True, [DMQb, I4])
                                p = PD[(2 * kt + g) % 4]
                                self.act(p[:, :, :], fs[g][:, :].rearrange("p (a t) -> p a t", a=4), AF.Exp, [fs[g]], [p])
                                for p4 in range(4):
                                    hc = 4 * g + p4
                                    self.mm(OD[g], OD[g][:, p4 * 65:(p4 + 1) * 65], p[:, p4, :], dv[:, kt, hc // 2, :],
                                            kt == 0 and p4 == 0, kt == i and p4 == 3, [p, dv])
                        for g in range(2):
                            self.cp("act", od[:, 4 * g:4 * g + 4, :], OD[g][:, 0:260].rearrange("p (a e) -> p a e", a=4), [OD[g]], [od])
                        if i + 1 < NT:
                            partA(s, i + 1)
                        if i >= 2:
                            S.op("dve", lambda e: e.tensor_reduce(out=bs[:, 0:1], in_=score[:, 0:nk], axis=AX.X, op=ALU.max),
                                 reads=[score.b], writes=[bs.b])
                            S.op("dve", lambda e: e.tensor_reduce(out=bs[:, 1:2], in_=score[:, 0:t0], axis=AX.X, op=ALU.min),
                                 reads=[score.b], writes=[bs.b])
                            self.tt("dve", bs[:, 2:3], bs[:, 0:1], bs[:, 1:2], ALU.subtract, [bs], [bs])
                            self.ts("dve", h2[:, :], P2H2[:, :], bs[:, 2:3], None, ALU.mult, None, [P2H2, bs], [h2])
                            self.ts("dve", hn[:, :], P2HN[:, :], bs[:, 2:3], None, ALU.mult, None, [P2HN, bs], [hn])
                            self.stt(bs[:, 3:4], bs[:, 2:3], 0.5, bs[:, 1:2], ALU.mult, ALU.add, [bs], [bs])
                            for n in range(NIT):
                                self.ts("dve", nm[:, 0:nk], score[:, 0:nk], bs[:, 3:4], 0.0, ALU.is_ge, ALU.add,
                                        [score, bs], [nm, bs], accum_out=bs[:, 4:5])
                                self.ts("dve", bs[:, 5:6], bs[:, 4:5], 255.5, h2[:, n + 1:n + 2], ALU.is_ge, ALU.mult, [bs, h2], [bs])
                                self.stt(bs[:, 3:4], bs[:, 5:6], hn[:, n + 1:n + 2], bs[:, 3:4], ALU.add, ALU.add, [bs, hn], [bs])
                            self.ts("dve", nm[:, 0:nk], score[:, 0:nk], bs[:, 3:4], NEG, ALU.is_lt, ALU.mult, [score, bs], [nm])
                        if self.cut(4):
                            continue
                        for kt in range(i + 1):
                            f = fb()
                            self.mm(f, f[:, :], akT2[:, kt * 128:(kt + 1) * 128], aqT2[:, :, :], True, False, [akT2, aqT2])
                            if i >= 2:
                                self.mm(f, f[:, :], nm[:, kt * 128:(kt + 1) * 128], I4[:, :, :], False, True, [nm, I4])
                            elif kt == i:
                                self.mm(f, f[:, :], DMQb[:, :], I4[:, :, :], False, True, [DMQb, I4])
                            p = PT[kt % 2]
                            self.act(p[:, :, :], f[:, :].rearrange("p (a t) -> p a t", a=4), AF.Exp, [f], [p])
                            for sl in range(4):
                                self.mm(OA, OA[:, sl * 65:(sl + 1) * 65], p[:, sl, :], av[:, kt, :], kt == 0 and sl == 0,
                                        kt == i and sl == 3, [p, av])
                        self.cp("act", oa[:, :, :], OA[:, 0:260].rearrange("p (a e) -> p a e", a=4), [OA], [oa])
                        S.op("dve", lambda e: e.reciprocal(out=rr[:, 0:4], in_=oa[:, :, 64]), reads=[oa.b], writes=[rr.b])
                        self.tt("dve", Y[:, 0:256].rearrange("p (h d) -> p h d", h=4), oa[:, :, 0:64],
                                rr[:, 0:4].unsqueeze(2).to_broadcast([128, 4, 64]), ALU.mult, [oa, rr], [Y])
                        S.op("dve", lambda e: e.reciprocal(out=rr[:, 8:16], in_=od[:, :, 64]), reads=[od.b], writes=[rr.b])
                        self.ts("dve", rr[:, 9:16:2], rr[:, 9:16:2], nlam[:, 0:1], None, ALU.mult, None, [rr, nlam], [rr])
                        self.tt("dve", tq[:, :].rearrange("p (a d) -> p a d", a=8), od[:, :, 0:64],
                                rr[:, 8:16].unsqueeze(2).to_broadcast([128, 8, 64]), ALU.mult, [od, rr], [tq])
                        tq4 = tq[:, :].rearrange("p (h c d) -> p h c d", h=4, c=2)
                        self.tt("pool", Y[:, 256:512].rearrange("p (h d) -> p h d", h=4), tq4[:, :, 0, :], tq4[:, :, 1, :],
                                ALU.add, [tq], [Y])
                        if self.cut(6):
                            continue
                        self.head_norm(Y, 8, False, self.hg[:, :].rearrange("p (h d) -> p h d", h=8), yo,
                                       yo[:, :].rearrange("p (h d) -> p h d", h=8), sq, st)
                        self.dma("sp", self.YAD[s, t0:t0 + 128, :], yo[:, :], "yo", [yo], [self.bYAD[s]])
                    S.barrier()
            S.barrier()


NIT = 16
_off = {}


def _lay():
    o = 0
    for name, n in (("IDENT", 128), ("INVA", 8), ("INVD", 4), ("INVB", 32), ("DMQ", 128), ("DMQ30", 128),
                    ("RMASK", 512), ("RXI", 512), ("RZ", 256), ("RG", 256),
                    ("BDM", 512), ("BD1", 512), ("BDS", 256), ("HM", 256), ("HMQ", 256), ("TRI", 128), ("SU", 128),
                    ("LM4", 512), ("UM4", 512), ("P2H2", NIT + 1), ("P2HN", NIT + 1)):
        _off[name] = (o, n)
        o += n
    return o


NCONST = _lay()
C_IDENT = 0


def make_consts():
    c = np.zeros((128, NCONST), np.float32)

    def put(name, arr):
        o, n = _off[name]
        c[:, o:o + n] = np.asarray(arr, np.float32).reshape(128, n)

    put("IDENT", np.eye(128))
    f32 = np.float32
    put("INVA", np.tile((f32(500000.0) ** (-np.arange(8, dtype=f32) / f32(8))).astype(f32)[None], (128, 1)))
    put("INVD", np.tile((f32(500000.0) ** (-np.arange(4, dtype=f32) / f32(4))).astype(f32)[None], (128, 1)))
    put("INVB", np.tile((f32(10000.0) ** (-np.arange(32, dtype=f32) / f32(32))).astype(f32)[None], (128, 1)))
    p = np.arange(128)
    q, k = p[:, None], p[None, :]
    inad = (q < 64) & (k >= 64)
    put("DMQ", np.where(inad, NEG, 0.0))
    put("DMQ30", np.where(inad, -1e30, 0.0))
    lg = np.log(1.0 - 2.0 ** (-5.0 - np.arange(4, dtype=np.float64)))
    j, i = p[:, None], p[None, :]
    rm = np.zeros((128, 4, 128))
    for h in range(4):
        m = np.exp(np.abs(i - j) * lg[h])
        m = np.where((j // 64) > (i // 64), 0.0, m)
        rm[:, h, :] = m
    put("RMASK", rm)
    xi = np.zeros((128, 4, 128))
    rg = np.zeros((128, 2, 128))
    hm = np.zeros((128, 2, 128))
    for h in range(4):
        xi[:, h, :] = np.exp((p + 1.0) * lg[h])[None, :]
        a_, b_ = h // 2, h % 2
        rg[64 * b_:64 * b_ + 64, a_, :] = np.exp(128.0 * lg[h])
    for b_ in range(2):
        hm[64 * b_:64 * b_ + 64, b_, :] = 1.0
    put("HM", hm)
    put("HMQ", hm * 0.125)
    put("RXI", xi)
    put("RG", rg)
    bd = np.zeros((128, 4, 128))
    bds = np.zeros((128, 4, 64))
    for h in range(4):
        bd[32 * h:32 * h + 32, h, :] = 1.0
        bds[32 * h:32 * h + 32, h, :] = 1.0
    put("BDM", bd * (32.0 ** -0.5))
    put("BD1", bd)
    put("BDS", bds)
    rz = np.zeros((128, 4, 64))
    for h in range(4):
        rz[:, h, :] = np.exp((127.0 - p) * lg[h])[:, None]
    put("RZ", rz)
    s_, t_ = p[:, None], p[None, :]
    put("TRI", np.where(s_ <= t_, -1.0 / 16.0, 0.0))
    put("SU", np.where(s_ > t_, -1.0 / 16.0, 0.0))
    lm = (t_ >= s_).astype(np.float64)
    um = ((t_ < s_) & ((s_ // 64) == (t_ // 64))).astype(np.float64)
    put("LM4", np.tile(lm[:, None, :], (1, 4, 1)))
    put("UM4", np.tile(um[:, None, :], (1, 4, 1)))
    h2 = np.array([2.0 * 2.0 ** -(n + 1) for n in range(NIT)] + [2.0 ** -NIT])
    hn = np.array([-(2.0 ** -(n + 1)) for n in range(NIT)] + [-(2.0 ** -NIT)])
    put("P2H2", np.tile(h2[None], (128, 1)))
    put("P2HN", np.tile(hn[None], (128, 1)))
    return c


def make_in_maps(inputs, nseq, T_, ncores):
    maps = []
    x = np.asarray(inputs["x"], np.float32)
    c = np.asarray(inputs["c"], np.float32)
    pos = np.asarray(inputs["positions"], np.int32)
    lam = np.stack([np.asarray(inputs[k], np.float32) for k in ("lam_q1", "lam_k1", "lam_q2", "lam_k2")], axis=1)
    consts = make_consts()
    shared = {k: np.ascontiguousarray(np.asarray(inputs[k], np.float32)) for k in
              ("mod_w", "mod_b", "attn_pre_g", "attn_post_g", "mlp_pre_g", "mlp_post_g", "w_in",
               "gla_wa2", "gla_ba", "head_norm_g", "w_out", "mlp_w1", "mlp_w2")}
    NT = T_ // 128
    for i in range(ncores):
        sl = slice(i * nseq, (i + 1) * nseq)
        cs = c[sl]
        cT = cs.reshape(nseq, 8, 128).transpose(2, 1, 0).reshape(128, 8 * nseq)
        ps_ = pos[sl].reshape(nseq, NT, 128).transpose(2, 0, 1).reshape(128, nseq * NT)
        m = dict(shared)
        m["x"] = np.ascontiguousarray(x[sl])
        m["cT"] = np.ascontiguousarray(cT)
        m["posT"] = np.ascontiguousarray(ps_)
        m["lam"] = np.ascontiguousarray(lam)
        m["consts"] = consts
        maps.append(m)
    return maps


def kernel(**inputs):
    B, T_, _ = inputs["x"].shape
    nseq = B // NCORES
    p = Prog(nseq, T_)
    nc = p.build()
    maps = make_in_maps(inputs, nseq, T_, NCORES)
    res = run_bass_kernel_spmd(nc, maps, core_ids=list(range(NCORES)))
    return np.concatenate([np.asarray(r["out"], np.float32) for r in res.results], axis=0)
```
